# Optimizing a Trainium2 kernel written in Bass

```python
import math
import jax
import jax.numpy as jnp
from jax import lax
import numpy as np

D_MODEL = 2048
BATCH = 8
SEQ = 4096
DEPTH = 4

N_A_LAYERS = DEPTH // 2
N_B_LAYERS = DEPTH - N_A_LAYERS

GDN_QK_HEADS = 16
GDN_V_HEADS = 32
GDN_HEAD_DIM = 128
GDN_CONV = 4
GDN_CHUNK = 64
GDN_QK_DIM = GDN_QK_HEADS * GDN_HEAD_DIM
GDN_V_DIM = GDN_V_HEADS * GDN_HEAD_DIM
GDN_CONV_DIM = 2 * GDN_QK_DIM + GDN_V_DIM
GDN_IN_DIM = GDN_CONV_DIM + GDN_V_DIM + 2 * GDN_V_HEADS

DIL_CONFIGS = ((128, 1), (512, 4), (2048, 16))
N_DIL = len(DIL_CONFIGS)
DIL_HEADS = 8
DIL_HEAD_DIM = 128
DIL_BLOCK = 128
DIL_KV_DIM = DIL_HEADS * DIL_HEAD_DIM
DIL_Q_DIM = N_DIL * DIL_KV_DIM

REL_BUCKETS = 32
REL_MAX_DIST = 2048

MOE_GROUPS = 4
MOE_EXPERTS_PER_GROUP = 8
MOE_EXPERTS = MOE_GROUPS * MOE_EXPERTS_PER_GROUP
MOE_TOP_K = 2
MOE_FF = 512
MOE_ROW_BLOCK = 256

DEEPNORM_ALPHA = (2 * DEPTH) ** 0.25
DEEPNORM_BETA = (8 * DEPTH) ** -0.25
LN_EPS = 1e-5
RMS_EPS = 1e-6
L2_EPS = 1e-6

kernel_name = 'yoco_gdn_dilated_hmoe_deepnorm'


def layer_norm(x, gain, bias):
    xf = x.astype(jnp.float32)
    mu = xf.mean(-1, keepdims=True)
    var = jnp.square(xf - mu).mean(-1, keepdims=True)
    y = (xf - mu) * lax.rsqrt(var + LN_EPS) * gain.astype(jnp.float32) + bias.astype(jnp.float32)
    return y.astype(x.dtype)


def l2_normalize(x):
    xf = x.astype(jnp.float32)
    return xf * lax.rsqrt(jnp.sum(xf * xf, -1, keepdims=True) + L2_EPS)


def causal_depthwise_conv(x, w):
    k, c = w.shape
    return lax.conv_general_dilated(
        x, w.astype(x.dtype)[:, None, :], window_strides=(1,), padding=((k - 1, 0),),
        dimension_numbers=('NWC', 'WIO', 'NWC'), feature_group_count=c)


def chunk_gated_delta_rule(q, k, v, g, beta):
    b, h, s, dk = q.shape
    dv = v.shape[-1]
    c = GDN_CHUNK
    n = s // c
    q = (q * dk ** -0.5).reshape(b, h, n, c, dk)
    k = k.reshape(b, h, n, c, dk)
    v = v.reshape(b, h, n, c, dv)
    beta = beta.reshape(b, h, n, c, 1)
    g = jnp.cumsum(g.reshape(b, h, n, c), axis=-1)
    causal = jnp.tril(jnp.ones((c, c), dtype=bool))
    strict = jnp.tril(jnp.ones((c, c), dtype=bool), -1)
    decay = jnp.exp(jnp.where(causal, g[..., :, None] - g[..., None, :], -jnp.inf))
    kb = k * beta
    lower = jnp.where(strict, jnp.einsum('bhnid,bhnjd->bhnij', kb, k) * decay, 0.0)
    tmat = lower + jnp.eye(c, dtype=lower.dtype)
    u = lax.linalg.triangular_solve(tmat, v * beta, left_side=True, lower=True)
    w = lax.linalg.triangular_solve(tmat, kb * jnp.exp(g)[..., None], left_side=True, lower=True)
    qk = jnp.einsum('bhnid,bhnjd->bhnij', q, k) * decay
    g_last = g[..., -1:]
    k_dec = k * jnp.exp(g_last - g)[..., None]
    q_dec = q * jnp.exp(g)[..., None]
    chunk_decay = jnp.exp(g_last)[..., None]

    def step(state, xs):
        qd, kd, wi, ui, qki, cd = xs
        v_new = ui - jnp.einsum('bhcd,bhde->bhce', wi, state)
        out = jnp.einsum('bhcd,bhde->bhce', qd, state) + jnp.einsum('bhij,bhje->bhie', qki, v_new)
        state = state * cd + jnp.einsum('bhcd,bhce->bhde', kd, v_new)
        return state, out

    xs = tuple(jnp.moveaxis(t, 2, 0) for t in (q_dec, k_dec, w, u, qk, chunk_decay))
    state0 = jnp.zeros((b, h, dk, dv), jnp.float32)
    _, out = lax.scan(step, state0, xs)
    return jnp.moveaxis(out, 0, 2).reshape(b, h, s, dv)


def gated_deltanet(x, w_in, conv_w, a_log, dt_bias, norm_w, w_out):
    b, s, _ = x.shape
    proj = x @ w_in
    o1 = GDN_CONV_DIM
    o2 = o1 + GDN_V_DIM
    o3 = o2 + GDN_V_HEADS
    qkv = jax.nn.silu(causal_depthwise_conv(proj[..., :o1], conv_w))
    z = proj[..., o1:o2].reshape(b, s, GDN_V_HEADS, GDN_HEAD_DIM).astype(jnp.float32)
    beta = jax.nn.sigmoid(proj[..., o2:o3].astype(jnp.float32))
    g = -jnp.exp(a_log.astype(jnp.float32)) * jax.nn.softplus(
        proj[..., o3:].astype(jnp.float32) + dt_bias.astype(jnp.float32))
    rep = GDN_V_HEADS // GDN_QK_HEADS
    q = jnp.repeat(l2_normalize(qkv[..., :GDN_QK_DIM].reshape(b, s, GDN_QK_HEADS, GDN_HEAD_DIM)), rep, axis=2)
    k = jnp.repeat(l2_normalize(qkv[..., GDN_QK_DIM:2 * GDN_QK_DIM].reshape(b, s, GDN_QK_HEADS, GDN_HEAD_DIM)), rep, axis=2)
    v = qkv[..., 2 * GDN_QK_DIM:].reshape(b, s, GDN_V_HEADS, GDN_HEAD_DIM).astype(jnp.float32)
    o = chunk_gated_delta_rule(q.transpose(0, 2, 1, 3), k.transpose(0, 2, 1, 3), v.transpose(0, 2, 1, 3),
                               g.transpose(0, 2, 1), beta.transpose(0, 2, 1))
    o = o.transpose(0, 2, 1, 3)
    o = o * lax.rsqrt(jnp.mean(o * o, -1, keepdims=True) + RMS_EPS) * norm_w.astype(jnp.float32)
    o = o * jax.nn.silu(z)
    return o.reshape(b, s, GDN_V_DIM).astype(x.dtype) @ w_out


def to_dilated_blocks(t, dil):
    b, s = t.shape[:2]
    rest = t.shape[2:]
    length = s // dil
    nb = -(-length // DIL_BLOCK)
    t = jnp.swapaxes(t.reshape(b, length, dil, *rest), 1, 2)
    pad = [(0, 0), (0, 0), (0, nb * DIL_BLOCK - length)] + [(0, 0)] * len(rest)
    return jnp.pad(t, pad).reshape(b, dil, nb, DIL_BLOCK, *rest)


def from_dilated_blocks(t, s):
    b, dil = t.shape[:2]
    rest = t.shape[4:]
    length = s // dil
    t = t.reshape(b, dil, -1, *rest)[:, :, :length]
    return jnp.swapaxes(t, 1, 2).reshape(b, s, *rest)


def with_previous_block(t):
    prev = jnp.pad(t, [(0, 0), (0, 0), (1, 0)] + [(0, 0)] * (t.ndim - 3))[:, :, :-1]
    return jnp.concatenate([prev, t], axis=3)


def t5_bucket(dist):
    exact = REL_BUCKETS // 2
    distf = jnp.maximum(dist, exact).astype(jnp.float32)
    large = exact + (jnp.log(distf / exact) / math.log(REL_MAX_DIST / exact)
                     * (REL_BUCKETS - exact)).astype(jnp.int32)
    return jnp.where(dist < exact, dist, jnp.minimum(large, REL_BUCKETS - 1))


def dilated_band(rel_bias_g, window, dil, nb):
    qi = jnp.arange(DIL_BLOCK)[:, None]
    kj = jnp.arange(2 * DIL_BLOCK)[None, :]
    steps = DIL_BLOCK + qi - kj
    bias = rel_bias_g[t5_bucket(jnp.maximum(steps, 0) * dil)]
    bias = jnp.transpose(bias, (2, 0, 1)).astype(jnp.float32)
    key_index = jnp.arange(nb)[:, None, None] * DIL_BLOCK - DIL_BLOCK + kj[None]
    mask = (steps >= 0) & (steps <= window // dil) & (key_index >= 0)
    return bias, mask


def dilated_group_attention(q, k_blk, v_blk, bias, mask, dil):
    s = q.shape[1]
    qb = to_dilated_blocks(q, dil)
    k_band = with_previous_block(k_blk)
    v_band = with_previous_block(v_blk)
    scores = jnp.einsum('bdnqhe,bdnkhe->bdnhqk', qb, k_band,
                        preferred_element_type=jnp.float32) * DIL_HEAD_DIM ** -0.5 + bias
    scores = jnp.where(mask[:, None], scores, -jnp.inf)
    m = scores.max(-1, keepdims=True)
    p = jnp.exp(scores - m)
    den = p.sum(-1)
    o = jnp.einsum('bdnhqk,bdnkhe->bdnqhe', p, v_band.astype(jnp.float32)) / jnp.swapaxes(den, -1, -2)[..., None]
    lse = jnp.swapaxes(m[..., 0] + jnp.log(den), -1, -2)
    return from_dilated_blocks(o, s), from_dilated_blocks(lse, s)


def shared_dilated_kv(h, w_k, w_v, rel_bias):
    b, s, _ = h.shape
    k = (h @ w_k).reshape(b, s, DIL_HEADS, DIL_HEAD_DIM)
    v = (h @ w_v).reshape(b, s, DIL_HEADS, DIL_HEAD_DIM)
    k_blks, v_blks, bands = [], [], []
    for gi, (window, dil) in enumerate(DIL_CONFIGS):
        kb = to_dilated_blocks(k, dil)
        k_blks.append(kb)
        v_blks.append(to_dilated_blocks(v, dil))
        bands.append(dilated_band(rel_bias[:, gi * DIL_HEADS:(gi + 1) * DIL_HEADS], window, dil, kb.shape[2]))
    return k_blks, v_blks, bands


def dilated_mixer(x, w_q, w_o, k_blks, v_blks, bands):
    b, s, _ = x.shape
    q = (x @ w_q).reshape(b, s, N_DIL, DIL_HEADS, DIL_HEAD_DIM)
    outs, lses = [], []
    for gi, (window, dil) in enumerate(DIL_CONFIGS):
        bias, mask = bands[gi]
        o, lse = dilated_group_attention(q[:, :, gi], k_blks[gi], v_blks[gi], bias, mask, dil)
        outs.append(o)
        lses.append(lse)
    wts = jax.nn.softmax(jnp.stack(lses), axis=0)
    o = jnp.sum(wts[..., None] * jnp.stack(outs), axis=0)
    return o.reshape(b, s, DIL_KV_DIM).astype(x.dtype) @ w_o


def grouped_expert_mlp(xt, expert_ids, gates, w1, w3, w2):
    t, d = xt.shape
    n_pairs = expert_ids.shape[0]
    token_of_pair = (jnp.arange(n_pairs) // MOE_TOP_K).astype(jnp.int32)
    onehot = (expert_ids[:, None] == jnp.arange(MOE_EXPERTS)[None, :]).astype(jnp.int32)
    counts = onehot.sum(0)
    rank = jnp.sum((jnp.cumsum(onehot, 0) - 1) * onehot, axis=1)
    padded = (counts + MOE_ROW_BLOCK - 1) // MOE_ROW_BLOCK * MOE_ROW_BLOCK
    seg_end = jnp.cumsum(padded)
    dest = (seg_end - padded)[expert_ids] + rank
    n_blocks = -(-n_pairs // MOE_ROW_BLOCK) + MOE_EXPERTS
    n_rows = n_blocks * MOE_ROW_BLOCK
    row_token = jnp.full((n_rows,), t, jnp.int32).at[dest].set(token_of_pair)
    row_gate = jnp.zeros((n_rows,), xt.dtype).at[dest].set(gates.astype(xt.dtype))
    block_expert = jnp.minimum(
        jnp.searchsorted(seg_end, jnp.arange(n_blocks) * MOE_ROW_BLOCK, side='right'), MOE_EXPERTS - 1)
    x_rows = jnp.concatenate([xt, jnp.zeros((1, d), xt.dtype)])[row_token].reshape(n_blocks, MOE_ROW_BLOCK, d)

    def expert_block(args):
        xb, e = args
        hmid = jax.nn.silu(xb @ w1[e]) * (xb @ w3[e])
        return hmid @ w2[e]

    y_rows = lax.map(expert_block, (x_rows, block_expert)).reshape(n_rows, d)
    return jnp.zeros((t + 1, d), y_rows.dtype).at[row_token].add(y_rows * row_gate[:, None])[:t]


def hierarchical_moe(x, w_group, b_group, w_expert, b_expert, w1, w3, w2):
    b, s, d = x.shape
    t = b * s
    xt = x.reshape(t, d)
    group_logits = (xt @ w_group + b_group).astype(jnp.float32)
    group_idx = jnp.argmax(group_logits, -1)
    group_gate = jnp.take_along_axis(jax.nn.softmax(group_logits, -1), group_idx[:, None], 1)
    expert_logits = (xt @ w_expert + b_expert).astype(jnp.float32).reshape(t, MOE_GROUPS, MOE_EXPERTS_PER_GROUP)
    local_logits = jnp.take_along_axis(expert_logits, group_idx[:, None, None], 1)[:, 0]
    top_p, top_i = lax.top_k(jax.nn.softmax(local_logits, -1), MOE_TOP_K)
    gates = group_gate * top_p / top_p.sum(-1, keepdims=True)
    expert_ids = (group_idx[:, None] * MOE_EXPERTS_PER_GROUP + top_i).astype(jnp.int32)
    y = grouped_expert_mlp(xt, expert_ids.reshape(-1), gates.reshape(-1), w1, w3, w2)
    return y.reshape(b, s, d)


def setup_inputs(seed: int = 0) -> dict:
    key = jax.random.key(seed)
    ks = jax.random.split(key, 24)
    f32 = jnp.float32

    def normal(k, shape, scale):
        return jax.random.normal(k, shape, f32) * scale

    dt = jnp.exp(jax.random.uniform(ks[4], (N_A_LAYERS, GDN_V_HEADS), f32, math.log(1e-3), math.log(1e-1)))
    return {
        'x': normal(ks[0], (BATCH, SEQ, D_MODEL), 1.0),
        'gdn_w_in': normal(ks[1], (N_A_LAYERS, D_MODEL, GDN_IN_DIM), D_MODEL ** -0.5),
        'gdn_conv': normal(ks[2], (N_A_LAYERS, GDN_CONV, GDN_CONV_DIM), GDN_CONV ** -0.5),
        'gdn_a_log': jnp.log(jax.random.uniform(ks[3], (N_A_LAYERS, GDN_V_HEADS), f32, 1.0, 16.0)),
        'gdn_dt_bias': dt + jnp.log(-jnp.expm1(-dt)),
        'gdn_norm': 1.0 + normal(ks[5], (N_A_LAYERS, GDN_HEAD_DIM), 0.02),
        'gdn_w_out': normal(ks[6], (N_A_LAYERS, GDN_V_DIM, D_MODEL), GDN_V_DIM ** -0.5 * DEEPNORM_BETA),
        'kv_w_k': normal(ks[7], (D_MODEL, DIL_KV_DIM), D_MODEL ** -0.5),
        'kv_w_v': normal(ks[8], (D_MODEL, DIL_KV_DIM), D_MODEL ** -0.5),
        'dil_w_q': normal(ks[9], (N_B_LAYERS, D_MODEL, DIL_Q_DIM), D_MODEL ** -0.5),
        'dil_w_o': normal(ks[10], (N_B_LAYERS, DIL_KV_DIM, D_MODEL), DIL_KV_DIM ** -0.5 * DEEPNORM_BETA),
        'rel_bias': normal(ks[11], (REL_BUCKETS, N_DIL * DIL_HEADS), 0.2),
        'ln_gain': 1.0 + normal(ks[12], (DEPTH, 2, D_MODEL), 0.02),
        'ln_bias': normal(ks[13], (DEPTH, 2, D_MODEL), 0.02),
        'moe_w_group': normal(ks[14], (DEPTH, D_MODEL, MOE_GROUPS), D_MODEL ** -0.5),
        'moe_b_group': normal(ks[15], (DEPTH, MOE_GROUPS), 0.01),
        'moe_w_expert': normal(ks[16], (DEPTH, D_MODEL, MOE_EXPERTS), D_MODEL ** -0.5),
        'moe_b_expert': normal(ks[17], (DEPTH, MOE_EXPERTS), 0.01),
        'moe_w1': normal(ks[18], (DEPTH, MOE_EXPERTS, D_MODEL, MOE_FF), D_MODEL ** -0.5),
        'moe_w3': normal(ks[19], (DEPTH, MOE_EXPERTS, D_MODEL, MOE_FF), D_MODEL ** -0.5),
        'moe_w2': normal(ks[20], (DEPTH, MOE_EXPERTS, MOE_FF, D_MODEL), MOE_FF ** -0.5 * DEEPNORM_BETA),
    }


def reference(x, gdn_w_in, gdn_conv, gdn_a_log, gdn_dt_bias, gdn_norm, gdn_w_out, kv_w_k, kv_w_v,
              dil_w_q, dil_w_o, rel_bias, ln_gain, ln_bias, moe_w_group, moe_b_group, moe_w_expert,
              moe_b_expert, moe_w1, moe_w3, moe_w2):
    h = x
    shared = None
    for layer in range(DEPTH):
        if layer < N_A_LAYERS:
            mix = gated_deltanet(h, gdn_w_in[layer], gdn_conv[layer], gdn_a_log[layer],
                                 gdn_dt_bias[layer], gdn_norm[layer], gdn_w_out[layer])
        else:
            if layer == N_A_LAYERS:
                shared = shared_dilated_kv(h, kv_w_k, kv_w_v, rel_bias)
            j = layer - N_A_LAYERS
            mix = dilated_mixer(h, dil_w_q[j], dil_w_o[j], *shared)
        h = layer_norm(DEEPNORM_ALPHA * h + mix, ln_gain[layer, 0], ln_bias[layer, 0])
        ffn = hierarchical_moe(h, moe_w_group[layer], moe_b_group[layer], moe_w_expert[layer],
                               moe_b_expert[layer], moe_w1[layer], moe_w3[layer], moe_w2[layer])
        h = layer_norm(DEEPNORM_ALPHA * h + ffn, ln_gain[layer, 1], ln_bias[layer, 1])
    return h
```

```python
import contextlib
import numpy as np
import concourse.bass as bass
import concourse.mybir as mybir
from concourse.bass_utils import run_bass_kernel_spmd

F32 = mybir.dt.float32
BF16 = mybir.dt.bfloat16
I32 = mybir.dt.int32
AF = mybir.ActivationFunctionType
ALU = mybir.AluOpType
AX = mybir.AxisListType

D = 2048
DEPTH = 4
NA = 2
ALPHA = (2 * DEPTH) ** 0.25
LN_EPS = 1e-5
NEG = -1.0e30
GDN_IN = 12352
NE = 32


class Buf:
    __slots__ = ("name", "w", "r", "const", "excl")

    def __init__(self, name, const=False, excl=False):
        self.name = name
        self.w = {}
        self.r = {}
        self.const = const
        self.excl = excl


class Ctx:
    ENG = ("pe", "dve", "act", "pool", "sp")
    ROT = 30000
    RING = 8

    def __init__(self, nc):
        self.nc = nc
        self.eng = {"pe": nc.tensor, "dve": nc.vector, "act": nc.scalar, "pool": nc.gpsimd, "sp": nc.sync}
        self.sem = {}
        self.cnt = {}
        self.nsem = 0
        self.seen = {e: {} for e in self.ENG}
        self.pending = {e: [] for e in self.ENG}
        self.ring = {}
        self.ring_pos = {}
        self.ninstr = 0
        for e in ("pe", "dve", "act", "pool"):
            self._newsem(e)
        for q in ("sp", "pool", "act"):
            self.ring[q] = [[self._alloc("dq_%s%d" % (q, i)), 0] for i in range(self.RING)]
            self.ring_pos[q] = 0

    def _alloc(self, name):
        self.nsem += 1
        return self.nc.alloc_semaphore(name=name)

    def _newsem(self, e):
        self.sem[e] = self._alloc("e_%s_%d" % (e, self.nsem))
        self.cnt[e] = 0

    def _wait(self, en, deps):
        seen = self.seen[en]
        need = {}
        for (sem, val) in deps:
            kk = id(sem)
            if en == "pe" and sem is self.sem["pe"]:
                continue
            if seen.get(kk, 0) >= val:
                continue
            if kk not in need or need[kk][1] < val:
                need[kk] = (sem, val)
        for kk, (sem, val) in need.items():
            self.eng[en].wait_ge(sem, val)
            seen[kk] = val
            self.ninstr += 1

    def _deps(self, reads, writes):
        deps = []
        for b in reads:
            deps.extend(b.w.values())
        for b in writes:
            deps.extend(b.w.values())
            deps.extend(b.r.values())
        return deps

    def _record(self, ev, reads, writes):
        kk = id(ev[0])
        for b in reads:
            if not b.const:
                b.r[kk] = ev
        for b in writes:
            b.w = {kk: ev}
            b.r = {}

    def op(self, en, fn, reads=(), writes=(), inc=True):
        ex = [b for b in reads if b.excl]
        if ex:
            reads = [b for b in reads if not b.excl]
            writes = list(writes) + ex
        self._wait(en, self._deps(reads, writes))
        ins = fn(self.eng[en])
        self.ninstr += 1
        if not inc:
            self.pending[en].append((tuple(reads), tuple(writes)))
            return ins
        if self.cnt[en] >= self.ROT:
            self._newsem(en)
        self.cnt[en] += 1
        ins.then_inc(self.sem[en], 1)
        ev = (self.sem[en], self.cnt[en])
        for (r, w) in self.pending[en]:
            self._record(ev, r, w)
        self.pending[en] = []
        self._record(ev, reads, writes)
        return ins

    def dma(self, q, out, in_, reads=(), writes=(), indirect=None, **kw):
        slot = self.ring[q][self.ring_pos[q] % self.RING]
        self.ring_pos[q] += 1
        deps = self._deps(reads, writes)
        if slot[1]:
            deps.append((slot[0], slot[1]))
        self._wait(q, deps)
        slot[1] += 16
        if indirect is None:
            ins = self.eng[q].dma_start(out=out, in_=in_, **kw)
        else:
            ins = self.eng[q].indirect_dma_start(out, indirect.get("out"), in_, indirect.get("in"), **kw)
        ins.then_inc(slot[0], 16)
        self.ninstr += 1
        self._record((slot[0], slot[1]), reads, writes)
        return ins

    def barrier(self):
        for e in self.ENG:
            assert not self.pending[e], "pending non-inc ops at barrier on %s" % e
        evs = []
        for e in ("pe", "dve", "act", "pool"):
            if self.cnt[e]:
                evs.append((self.sem[e], self.cnt[e]))
        for q in self.ring:
            for s, v in self.ring[q]:
                if v:
                    evs.append((s, v))
        for e in self.ENG:
            self._wait(e, evs)


class Stage:
    def __init__(self, P):
        self.k = P.k
        self.nc = P.nc
        self.es = contextlib.ExitStack()

    def __enter__(self):
        self.es.__enter__()
        return self

    def __exit__(self, *a):
        self.k.barrier()
        return self.es.__exit__(*a)

    def sb(self, name, shape, dtype=F32, const=False):
        self.k.ninstr += 1
        t = self.es.enter_context(self.nc.sbuf_tensor("%s_%d" % (name, self.k.ninstr), list(shape), dtype))
        return t, Buf(name, const)

    def ps(self, name, shape, dtype=F32):
        self.k.ninstr += 1
        n = 1
        for x in shape[1:]:
            n *= x
        full = 512 if dtype == F32 else 1024
        assert n <= full
        t = self.es.enter_context(self.nc.psum_tensor("%s_%d" % (name, self.k.ninstr), [128, full], dtype))
        return t, Buf(name, excl=True)


class Prog:
    def __init__(self, T, mode="full", dbg=()):
        self.T = T
        self.NT = T // 128
        self.mode = mode
        self.nc = bass.Bass("TRN2", target_bir_lowering=False)
        self.k = Ctx(self.nc)
        self.dr = {}
        self.db = {}
        self.dbg = set(dbg)
        self.gstack = contextlib.ExitStack()

    def din(self, name, shape, dtype=F32):
        t = self.nc.dram_tensor(name, list(shape), dtype, kind="ExternalInput")
        self.dr[name] = t.ap()
        self.db[name] = Buf(name, const=True)
        return self.dr[name]

    def dscr(self, name, shape, dtype=F32, out=False):
        kind = "ExternalOutput" if (out or name in self.dbg) else "Internal"
        t = self.nc.dram_tensor(name, list(shape), dtype, kind=kind)
        self.dr[name] = t.ap()
        self.db[name] = Buf(name)
        return self.dr[name]

    def gsb(self, name, shape, dtype=F32, const=True):
        t = self.gstack.enter_context(self.nc.sbuf_tensor("g_" + name, list(shape), dtype))
        return t, Buf(name, const)

    def setup_consts(self):
        k = self.k
        self.ident, self.identb = self.gsb("ident", [128, 128])
        self.identh, self.identhb = self.gsb("identh", [128, 128], BF16)
        self.ones, self.onesb = self.gsb("ones", [128, 128])
        dd, ddb = self.gsb("dd", [128, 128], F32, const=False)
        k.op("pool", lambda e: e.iota(dd[:], [[1, 128]], base=0, channel_multiplier=-1,
                                      allow_small_or_imprecise_dtypes=True), writes=[ddb])
        k.op("dve", lambda e: e.tensor_scalar(self.ident[:], dd[:], 0.0, None, ALU.is_equal), reads=[ddb], writes=[self.identb])
        k.op("dve", lambda e: e.tensor_copy(self.identh[:], self.ident[:]), reads=[self.identb], writes=[self.identhb])
        k.op("dve", lambda e: e.memset(self.ones[:], 1.0), writes=[self.onesb])
        self.dd, self.ddb = dd, ddb
        k.barrier()
        for b in (self.identb, self.identhb, self.onesb, self.ddb):
            b.const = True
            b.r = {}

    def ln_tile(self, st, tiles, layer, which, s_src, s_srcb, h_dram_tile, out_tile_row0, router, final=False):
        k = self.k
        t = tiles
        hh, hhb = t["h"]
        s, sb_ = t["s"]
        stats, statsb = t["stats"]
        mv, mvb = t["mv"]
        rstd, rstdb = t["rstd"]
        k.dma("sp", hh[:], h_dram_tile, reads=[self.Hb[out_tile_row0]], writes=[hhb])
        k.op("dve", lambda e: e.scalar_tensor_tensor(s[:], hh[:], ALPHA, s_src, ALU.mult, ALU.add),
             reads=[hhb, s_srcb], writes=[sb_])
        for c in range(4):
            k.op("dve", lambda e: e.bn_stats(stats[:, c * 6:(c + 1) * 6], s[:, c * 512:(c + 1) * 512]), reads=[sb_], writes=[statsb])
        k.op("dve", lambda e: e.bn_aggr(mv[:], stats[:]), reads=[statsb], writes=[mvb])
        k.op("act", lambda e: e.activation(rstd[:], mv[:, 1:2], AF.Sqrt, bias=LN_EPS), reads=[mvb], writes=[rstdb])
        k.op("dve", lambda e: e.reciprocal(rstd[:], rstd[:]), reads=[rstdb], writes=[rstdb])
        xn, xnb = t["xn"]
        k.op("dve", lambda e: e.tensor_scalar(xn[:], s[:], mv[:, 0:1], rstd[:, 0:1], ALU.subtract, ALU.mult),
             reads=[sb_, mvb, rstdb], writes=[xnb])
        gain, gainb = t["gain"]
        bias, biasb = t["bias"]
        k.op("dve", lambda e: e.tensor_tensor(xn[:], xn[:], gain[:], ALU.mult), reads=[xnb, gainb], writes=[xnb])
        k.op("dve", lambda e: e.tensor_tensor(xn[:], xn[:], bias[:], ALU.add), reads=[xnb, biasb], writes=[xnb])
        return xn, xnb

    def mm_tok(self, name, XT, XTb, K, W, Wb, N, OUT, OUTb, out_dtype=F32):
        k = self.k
        T = self.T
        KC = K // 128
        with Stage(self) as st:
            wts = [st.sb("w%d" % i, [128, KC, 512], BF16) for i in range(2)]
            xgs = [st.sb("xg%d" % i, [128, KC, 512], BF16) for i in range(2)]
            ots = [st.sb("ot%d" % i, [128, 4, 512], out_dtype) for i in range(2)]
            pss = [st.ps("ps%d" % i, [128, 512]) for i in range(4)]
            nblocks = [(n0, min(512, N - n0)) for n0 in range(0, N, 512)]
            ntg = T // 512

            def load_w(i):
                n0, nw = nblocks[i]
                wt, wtb = wts[i % 2]
                for c0 in range(0, KC, 8):
                    c1 = min(KC, c0 + 8)
                    src = W[c0 * 128:c1 * 128, n0:n0 + nw].rearrange("(kc p) n -> p kc n", p=128)
                    k.dma("pool", wt[:, c0:c1, 0:nw], src, reads=[Wb], writes=[wtb])

            cnt = 0

            def load_x(j):
                tg = j % ntg
                xg, xgb = xgs[j % 2]
                src = XT[:, tg * 512:(tg + 1) * 512].rearrange("(kc p) t -> p kc t", p=128)
                k.dma("sp", xg[:], src, reads=[XTb], writes=[xgb])

            load_w(0)
            load_x(0)
            j = 0
            pi = 0
            for bi, (n0, nw) in enumerate(nblocks):
                if bi + 1 < len(nblocks):
                    load_w(bi + 1)
                wt, wtb = wts[bi % 2]
                for tg in range(ntg):
                    if j + 1 < len(nblocks) * ntg:
                        load_x(j + 1)
                    xg, xgb = xgs[j % 2]
                    ot, otb = ots[j % 2]
                    for tt in range(4):
                        ps, psb = pss[pi % 4]
                        pi += 1
                        for kc in range(KC):
                            k.op("pe", lambda e: e.matmul(ps[:, 0:nw], xg[:, kc, tt * 128:(tt + 1) * 128], wt[:, kc, 0:nw],
                                                          start=(kc == 0), stop=(kc == KC - 1)),
                                 reads=[xgb, wtb], writes=[psb], inc=(kc == KC - 1))
                        if tt % 2 == 0:
                            k.op("act", lambda e: e.copy(ot[:, tt, 0:nw], ps[:, 0:nw]), reads=[psb], writes=[otb])
                        else:
                            k.op("dve", lambda e: e.tensor_copy(ot[:, tt, 0:nw], ps[:, 0:nw]), reads=[psb], writes=[otb])
                    dst = OUT[tg * 512:(tg + 1) * 512, n0:n0 + nw].rearrange("(tt p) n -> p tt n", p=128)
                    k.dma("sp", dst, ot[:, :, 0:nw], reads=[otb], writes=[OUTb])
                    j += 1

    def ln_stage(self, layer, which, Yname, router, final_out=None):
        k = self.k
        T = self.T
        dr, db = self.dr, self.db
        with Stage(self) as st:
            tiles = self.alloc_ln_tiles(st, layer, which, router)
            yt = [st.sb("yt%d" % i, [128, D]) for i in range(2)]

            def load_y(i):
                y, yb = yt[i % 2]
                k.dma("sp", y[:], dr[Yname][i * 128:(i + 1) * 128, :], reads=[db[Yname]], writes=[yb])

            load_y(0)
            for ti in range(self.NT):
                if ti + 1 < self.NT:
                    load_y(ti + 1)
                y, yb = yt[ti % 2]
                self.ln_full_tile(st, tiles, layer, which, y[:], yb, ti, router)

    def alloc_ln_tiles(self, st, layer, which, router):
        k = self.k
        dr, db = self.dr, self.db
        t = {}
        t["h"] = st.sb("h", [128, D])
        t["s"] = t["h"]
        t["stats"] = st.sb("stats", [128, 24])
        t["mv"] = st.sb("mv", [128, 2])
        t["rstd"] = st.sb("rstd", [128, 1])
        t["xn"] = t["h"]
        t["gain"] = st.sb("gain", [128, D], const=True)
        t["bias"] = st.sb("bias", [128, D], const=True)
        t["xT32"] = st.sb("xT32", [128, 16, 128]) if router else (None, None)
        t["xTb"] = st.sb("xTb", [128, 16, 512], BF16)
        t["pst"] = [st.ps("pst%d" % i, [128, 512]) for i in range(2)]
        k.dma("sp", t["gain"][0][:], dr["ln_gain"][layer, which], reads=[db["ln_gain"]], writes=[t["gain"][1]])
        k.dma("sp", t["bias"][0][:], dr["ln_bias"][layer, which], reads=[db["ln_bias"]], writes=[t["bias"][1]])
        if router:
            t["wr"] = st.sb("wr", [128, 16, 36], const=True)
            t["br"] = st.sb("br", [128, 36], const=True)
            k.dma("sp", t["wr"][0][:], dr["w_router"][layer].rearrange("(kc p) n -> p kc n", p=128),
                  reads=[db["w_router"]], writes=[t["wr"][1]])
            k.dma("sp", t["br"][0][:], dr["b_router"][layer], reads=[db["b_router"]], writes=[t["br"][1]])
            t["psr"] = st.ps("psr", [128, 36])
            for nm, shp in (("L", [128, 36]), ("mg", [128, 1]), ("ohg", [128, 4]), ("eg", [128, 4]), ("sg", [128, 1]),
                            ("ll", [128, 8]), ("m1", [128, 1]), ("oh1", [128, 8]), ("ll2", [128, 8]), ("m2", [128, 1]),
                            ("oh2", [128, 8]), ("r", [128, 1]), ("den", [128, 1]), ("g1", [128, 1]), ("g2", [128, 1]),
                            ("gl", [128, 8]), ("G", [128, 4, 8])):
                t[nm] = st.sb("r_" + nm, shp)
        return t

    def ln_full_tile(self, st, t, layer, which, ysrc, ysrcb, ti, router):
        k = self.k
        dr, db = self.dr, self.db
        xn, xnb = self.ln_tile(st, t, layer, which, ysrc, ysrcb, dr["H"][ti * 128:(ti + 1) * 128, :], ti, router)
        last = (layer == DEPTH - 1 and which == 1)
        if last:
            k.dma("sp", dr["out"][ti * 128:(ti + 1) * 128, :], xn[:], reads=[xnb], writes=[db["out"]])
            return
        k.dma("sp", dr["H"][ti * 128:(ti + 1) * 128, :], xn[:], reads=[xnb], writes=[self.Hb[ti]])
        xT32, xT32b = t["xT32"]
        xTb, xTbb = t["xTb"]
        q = ti % 4
        for c4 in range(4):
            ps, psb = t["pst"][c4 % 2]
            for c in range(4):
                kc = c4 * 4 + c
                k.op("pe", lambda e: e.transpose(ps[:, c * 128:(c + 1) * 128], xn[:, kc * 128:(kc + 1) * 128], self.ident[:]),
                     reads=[xnb, self.identb], writes=[psb], inc=(c == 3))
            if router:
                k.op("act", lambda e: e.copy(xT32[:, c4 * 4:(c4 + 1) * 4, :], ps[:].rearrange("p (a b) -> p a b", a=4)), reads=[psb], writes=[xT32b])
            k.op("dve", lambda e: e.tensor_copy(xTb[:, c4 * 4:(c4 + 1) * 4, q * 128:(q + 1) * 128], ps[:].rearrange("p (a b) -> p a b", a=4)),
                 reads=[psb], writes=[xTbb])
        if q == 3:
            tg = ti // 4
            dst = dr["HT"][:, tg * 512:(tg + 1) * 512].rearrange("(kc p) t -> p kc t", p=128)
            k.dma("sp", dst, xTb[:], reads=[xTbb], writes=[db["HT"]])
        if router:
            self.router_tile(t, layer, ti)

    def router_tile(self, t, layer, ti):
        k = self.k
        dr, db = self.dr, self.db
        xT32, xT32b = t["xT32"]
        wr, wrb = t["wr"]
        psr, psrb = t["psr"]
        for kc in range(16):
            k.op("pe", lambda e: e.matmul(psr[:, 0:36], xT32[:, kc, :], wr[:, kc, :], start=(kc == 0), stop=(kc == 15)),
                 reads=[xT32b, wrb], writes=[psrb], inc=(kc == 15))
        g = lambda n: t[n][0]
        b = lambda n: t[n][1]

        def dv(fn, reads, writes, en="dve"):
            k.op(en, fn, reads=[b(n) if isinstance(n, str) else n for n in reads], writes=[b(n) for n in writes])

        dv(lambda e: e.tensor_tensor(g("L")[:], psr[:, 0:36], t["br"][0][:], ALU.add), [psrb, t["br"][1]], ["L"])
        L = g("L")
        dv(lambda e: e.tensor_reduce(g("mg")[:], L[:, 0:4], AX.X, ALU.max), ["L"], ["mg"])
        dv(lambda e: e.tensor_scalar(g("ohg")[:], L[:, 0:4], g("mg")[:, 0:1], None, ALU.is_equal), ["L", "mg"], ["ohg"])
        dv(lambda e: e.tensor_scalar(g("eg")[:], L[:, 0:4], g("mg")[:, 0:1], None, ALU.subtract), ["L", "mg"], ["eg"])
        dv(lambda e: e.activation(g("eg")[:], g("eg")[:], AF.Exp), ["eg"], ["eg"], en="act")
        dv(lambda e: e.tensor_reduce(g("sg")[:], g("eg")[:], AX.X, ALU.add), ["eg"], ["sg"])
        for gi in range(4):
            if gi == 0:
                dv(lambda e: e.tensor_scalar(g("ll")[:], L[:, 4:12], g("ohg")[:, 0:1], None, ALU.mult), ["L", "ohg"], ["ll"])
            else:
                dv(lambda e: e.scalar_tensor_tensor(g("ll")[:], L[:, 4 + 8 * gi:12 + 8 * gi], g("ohg")[:, gi:gi + 1], g("ll")[:],
                                                    ALU.mult, ALU.add), ["L", "ohg", "ll"], ["ll"])
        dv(lambda e: e.tensor_reduce(g("m1")[:], g("ll")[:], AX.X, ALU.max), ["ll"], ["m1"])
        dv(lambda e: e.tensor_scalar(g("oh1")[:], g("ll")[:], g("m1")[:, 0:1], None, ALU.is_equal), ["ll", "m1"], ["oh1"])
        dv(lambda e: e.scalar_tensor_tensor(g("ll2")[:], g("oh1")[:], NEG, g("ll")[:], ALU.mult, ALU.add), ["oh1", "ll"], ["ll2"])
        dv(lambda e: e.tensor_reduce(g("m2")[:], g("ll2")[:], AX.X, ALU.max), ["ll2"], ["m2"])
        dv(lambda e: e.tensor_scalar(g("oh2")[:], g("ll2")[:], g("m2")[:, 0:1], None, ALU.is_equal), ["ll2", "m2"], ["oh2"])
        dv(lambda e: e.tensor_tensor(g("r")[:], g("m2")[:], g("m1")[:], ALU.subtract), ["m1", "m2"], ["r"])
        dv(lambda e: e.activation(g("r")[:], g("r")[:], AF.Exp), ["r"], ["r"], en="act")
        dv(lambda e: e.scalar_tensor_tensor(g("den")[:], g("r")[:], 1.0, g("sg")[:], ALU.add, ALU.mult), ["r", "sg"], ["den"])
        dv(lambda e: e.reciprocal(g("g1")[:], g("den")[:]), ["den"], ["g1"])
        dv(lambda e: e.tensor_tensor(g("g2")[:], g("g1")[:], g("r")[:], ALU.mult), ["g1", "r"], ["g2"])
        dv(lambda e: e.tensor_scalar(g("gl")[:], g("oh1")[:], g("g1")[:, 0:1], None, ALU.mult), ["oh1", "g1"], ["gl"])
        dv(lambda e: e.scalar_tensor_tensor(g("gl")[:], g("oh2")[:], g("g2")[:, 0:1], g("gl")[:], ALU.mult, ALU.add),
           ["oh2", "g2", "gl"], ["gl"])
        for gi in range(4):
            dv(lambda e: e.tensor_scalar(g("G")[:, gi, :], g("gl")[:], g("ohg")[:, gi:gi + 1], None, ALU.mult), ["gl", "ohg"], ["G"])
        k.dma("sp", dr["G"][ti * 128:(ti + 1) * 128, :], g("G")[:].rearrange("p a b -> p (a b)"), reads=[b("G")], writes=[db["G"]])

    def moe_stage(self, layer):
        k = self.k
        T = self.T
        dr, db = self.dr, self.db
        ntg = T // 512
        with Stage(self) as st:
            tiles = self.alloc_ln_tiles(st, layer, 1, False)
            w13 = [st.sb("w13_%d" % i, [128, 16, 1024], BF16) for i in range(2)]
            w2 = [st.sb("w2_%d" % i, [128, 4, 2048], BF16) for i in range(2)]
            xg, xgb = st.sb("xg", [128, 16, 512], BF16)
            gt, gtb = st.sb("gt", [128, 4, 32])
            yacc, yaccb = st.sb("yacc", [128, 4, D])
            sil = [st.sb("sil%d" % i, [128, 512]) for i in range(2)]
            hT, hTb = st.sb("hT", [128, 4, 512], BF16)
            ps1 = [st.ps("ps1_%d" % i, [128, 512]) for i in range(2)]
            ps3 = [st.ps("ps3_%d" % i, [128, 512]) for i in range(2)]
            ps2 = [st.ps("ps2_%d" % i, [128, 512]) for i in range(2)]

            def load_w(j):
                e_ = j % NE
                a, ab = w13[j % 2]
                c, cb = w2[j % 2]
                for half, nm in ((0, "moe_w1"), (1, "moe_w3")):
                    for c0 in (0, 8):
                        src = dr[nm][layer, e_, c0 * 128:(c0 + 8) * 128, :].rearrange("(kc p) n -> p kc n", p=128)
                        k.dma("pool", a[:, c0:c0 + 8, half * 512:(half + 1) * 512], src, reads=[db[nm]], writes=[ab])
                src = dr["moe_w2"][layer, e_].rearrange("(fc p) n -> p fc n", p=128)
                k.dma("pool", c[:], src, reads=[db["moe_w2"]], writes=[cb])

            total = ntg * NE
            load_w(0)
            j = 0
            p2 = 0
            for tg in range(ntg):
                k.dma("sp", xg[:], dr["HT"][:, tg * 512:(tg + 1) * 512].rearrange("(kc p) t -> p kc t", p=128),
                      reads=[db["HT"]], writes=[xgb])
                k.dma("sp", gt[:], dr["G"][tg * 512:(tg + 1) * 512, :].rearrange("(tt p) n -> p tt n", p=128),
                      reads=[db["G"]], writes=[gtb])
                for e_ in range(NE):
                    if j + 1 < total:
                        load_w(j + 1)
                    a, ab = w13[j % 2]
                    c, cb = w2[j % 2]
                    for fc in range(4):
                        p1, p1b = ps1[fc % 2]
                        p3, p3b = ps3[fc % 2]
                        for kc in range(16):
                            k.op("pe", lambda e: e.matmul(p1[:], a[:, kc, fc * 128:(fc + 1) * 128], xg[:, kc, :],
                                                          start=(kc == 0), stop=(kc == 15)),
                                 reads=[ab, xgb], writes=[p1b], inc=(kc == 15))
                        for kc in range(16):
                            k.op("pe", lambda e: e.matmul(p3[:], a[:, kc, 512 + fc * 128:512 + (fc + 1) * 128], xg[:, kc, :],
                                                          start=(kc == 0), stop=(kc == 15)),
                                 reads=[ab, xgb], writes=[p3b], inc=(kc == 15))
                        sl, slb = sil[fc % 2]
                        k.op("act", lambda e: e.activation(sl[:], p1[:], AF.Silu), reads=[p1b], writes=[slb])
                        k.op("dve", lambda e: e.tensor_tensor(hT[:, fc, :], sl[:], p3[:], ALU.mult),
                             reads=[slb, p3b], writes=[hTb])
                    for tt in range(4):
                        for dbk in range(4):
                            pp, ppb = ps2[p2 % 2]
                            p2 += 1
                            for fc in range(4):
                                k.op("pe", lambda e: e.matmul(pp[:], hT[:, fc, tt * 128:(tt + 1) * 128],
                                                              c[:, fc, dbk * 512:(dbk + 1) * 512],
                                                              start=(fc == 0), stop=(fc == 3)),
                                     reads=[hTb, cb], writes=[ppb], inc=(fc == 3))
                            ysl = yacc[:, tt, dbk * 512:(dbk + 1) * 512]
                            if e_ == 0:
                                k.op("dve", lambda e: e.tensor_scalar(ysl, pp[:], gt[:, tt, e_:e_ + 1], None, ALU.mult),
                                     reads=[ppb, gtb], writes=[yaccb])
                            else:
                                k.op("dve", lambda e: e.scalar_tensor_tensor(ysl, pp[:], gt[:, tt, e_:e_ + 1], ysl, ALU.mult, ALU.add),
                                     reads=[ppb, gtb, yaccb], writes=[yaccb])
                    j += 1
                for tt in range(4):
                    self.ln_full_tile(st, tiles, layer, 1, yacc[:, tt, :], yaccb, tg * 4 + tt, False)


    def declare_gdn(self):
        T = self.T
        self.din("gdn_w_in", [NA, D, GDN_IN])
        self.din("gdn_conv", [NA, 64, 128, 4])
        self.din("gdn_alog", [NA, 128, 32])
        self.din("gdn_dtb", [NA, 128, 32])
        self.din("gdn_norm", [NA, 128, 128])
        self.din("gdn_w_out", [NA, 4096, D])
        self.dscr("qT", [2048, T], BF16)
        self.dscr("kT", [2048, T], BF16)
        self.dscr("ktok", [T, 2048], BF16)
        self.dscr("vtok", [T, 4096], BF16)
        self.dscr("zba", [T, 4160])
        self.dscr("OT", [4096, T], BF16)

    def gdn_qkv(self, l):
        k = self.k
        T = self.T
        dr, db = self.dr, self.db
        TH = min(T, 2048)
        nh = T // TH
        ntg = TH // 512
        ntt = TH // 128
        with Stage(self) as st:
            xT, xTb = st.sb("xT", [128, 16, TH], BF16)
            wts = [st.sb("w%d" % i, [128, 16, 128], BF16) for i in range(2)]
            cws = [st.sb("cw%d" % i, [128, 4]) for i in range(2)]
            raw, rawb = st.sb("raw", [128, TH + 4])
            acc, accb = st.sb("acc", [128, TH])
            sq, sqb = st.sb("sq", [128, TH])
            rs, rsb = st.sb("rs", [128, 512])
            ob, obb = st.sb("ob", [128, TH], BF16)
            tk, tkb = st.sb("tk", [128, ntt, 128], BF16)
            halo, halob = st.sb("halo", [128, 64, 4])
            pss = [st.ps("ps%d" % i, [128, 512]) for i in range(2)]
            psn, psnb = st.ps("psn", [128, 512])
            pst = [st.ps("pst%d" % i, [128, 1024], BF16) for i in range(2)]
            Wl = dr["gdn_w_in"][l]

            def load_w(j):
                nb = j % 64
                wt, wtb = wts[j % 2]
                cw, cwb = cws[j % 2]
                for c0 in (0, 8):
                    src = Wl[c0 * 128:(c0 + 8) * 128, nb * 128:(nb + 1) * 128].rearrange("(kc p) n -> p kc n", p=128)
                    k.dma("pool", wt[:, c0:c0 + 8, :], src, reads=[db["gdn_w_in"]], writes=[wtb])
                k.dma("sp", cw[:], dr["gdn_conv"][l, nb], reads=[db["gdn_conv"]], writes=[cwb])

            j = 0
            pi = 0
            load_w(0)
            for half in range(nh):
                t0 = half * TH
                for kc in range(16):
                    k.dma("sp", xT[:, kc, :], dr["HT"][kc * 128:(kc + 1) * 128, t0:t0 + TH], reads=[db["HT"]], writes=[xTb])
                for nb in range(64):
                    if j + 1 < nh * 64:
                        load_w(j + 1)
                    wt, wtb = wts[j % 2]
                    cw, cwb = cws[j % 2]
                    j += 1
                    if half == 0:
                        k.op("dve", lambda e: e.memset(raw[:, 0:3], 0.0), writes=[rawb])
                    else:
                        k.op("dve", lambda e: e.tensor_copy(raw[:, 0:3], halo[:, nb, 0:3]), reads=[halob], writes=[rawb])
                    for tg in range(ntg):
                        ps, psb = pss[pi % 2]
                        pi += 1
                        for kc in range(16):
                            k.op("pe", lambda e: e.matmul(ps[:], wt[:, kc, :], xT[:, kc, tg * 512:(tg + 1) * 512],
                                                          start=(kc == 0), stop=(kc == 15)),
                                 reads=[wtb, xTb], writes=[psb], inc=(kc == 15))
                        k.op("act", lambda e: e.copy(raw[:, 3 + tg * 512:3 + (tg + 1) * 512], ps[:]), reads=[psb], writes=[rawb])
                    if half + 1 < nh:
                        k.op("dve", lambda e: e.tensor_copy(halo[:, nb, 0:3], raw[:, TH:TH + 3]), reads=[rawb], writes=[halob])
                    k.op("dve", lambda e: e.tensor_scalar(acc[:], raw[:, 0:TH], cw[:, 0:1], None, ALU.mult),
                         reads=[rawb, cwb], writes=[accb])
                    for jj in range(1, 4):
                        k.op("dve", lambda e: e.scalar_tensor_tensor(acc[:], raw[:, jj:jj + TH], cw[:, jj:jj + 1], acc[:], ALU.mult, ALU.add),
                             reads=[rawb, cwb, accb], writes=[accb])
                    k.op("act", lambda e: e.activation(acc[:], acc[:], AF.Silu), reads=[accb], writes=[accb])
                    if nb < 32:
                        k.op("act", lambda e: e.activation(sq[:], acc[:], AF.Square), reads=[accb], writes=[sqb])
                        for tg in range(ntg):
                            sl = slice(tg * 512, (tg + 1) * 512)
                            k.op("pe", lambda e: e.matmul(psn[:], self.ones[:], sq[:, sl], start=True, stop=True),
                                 reads=[sqb, self.onesb], writes=[psnb])
                            k.op("act", lambda e: e.activation(rs[:], psn[:], AF.Sqrt, bias=1e-6), reads=[psnb], writes=[rsb])
                            k.op("dve", lambda e: e.reciprocal(rs[:], rs[:]), reads=[rsb], writes=[rsb])
                            sc = (128.0 ** -0.5) if nb < 16 else 1.0
                            k.op("dve", lambda e: e.scalar_tensor_tensor(ob[:, sl], acc[:, sl], sc, rs[:], ALU.mult, ALU.mult),
                                 reads=[accb, rsb], writes=[obb])
                        dst = dr["qT"] if nb < 16 else dr["kT"]
                        dstb = db["qT"] if nb < 16 else db["kT"]
                        r0 = (nb % 16) * 128
                        k.dma("sp", dst[r0:r0 + 128, t0:t0 + TH], ob[:], reads=[obb], writes=[dstb])
                    else:
                        k.op("dve", lambda e: e.tensor_copy(ob[:], acc[:]), reads=[accb], writes=[obb])
                    if nb >= 16:
                        for t8 in range(0, ntt, 8):
                            pt, ptb = pst[(t8 // 8) % 2]
                            for tt in range(t8, min(ntt, t8 + 8)):
                                k.op("pe", lambda e: e.transpose(pt[:, (tt - t8) * 128:(tt - t8 + 1) * 128], ob[:, tt * 128:(tt + 1) * 128], self.identh[:]),
                                     reads=[obb, self.identhb], writes=[ptb], inc=(tt == min(ntt, t8 + 8) - 1))
                            n8 = min(ntt, t8 + 8) - t8
                            k.op("act", lambda e: e.copy(tk[:, t8:t8 + n8, :], pt[:, 0:n8 * 128].rearrange("p (a b) -> p a b", b=128)),
                                 reads=[ptb], writes=[tkb])
                        if nb < 32:
                            dst = dr["ktok"][t0:t0 + TH, (nb - 16) * 128:(nb - 15) * 128]
                            dstb = db["ktok"]
                        else:
                            dst = dr["vtok"][t0:t0 + TH, (nb - 32) * 128:(nb - 31) * 128]
                            dstb = db["vtok"]
                        k.dma("sp", dst.rearrange("(tt p) e -> p tt e", p=128), tk[:], reads=[tkb], writes=[dstb])


    def gdn_core(self, l):
        k = self.k
        T = self.T
        NT = self.NT
        dr, db = self.dr, self.db
        NS = 8
        with Stage(self) as st:
            tri, trib = st.sb("tri", [128, 128], const=True)
            maskneg, masknegb = st.sb("maskneg", [128, 128], const=True)
            strict, strictb = st.sb("strict", [128, 128], F32, const=True)
            Eall, Eallb = st.sb("Eall", [32, 32, 128], const=True)
            normw, normwb = st.sb("normw", [128, 128], const=True)
            k.op("dve", lambda e: e.tensor_scalar(tri[:], self.dd[:], 0.0, None, ALU.is_ge), reads=[self.ddb], writes=[trib])
            k.op("dve", lambda e: e.tensor_scalar(maskneg[:], self.dd[:], 0.0, NEG, ALU.is_lt, ALU.mult), reads=[self.ddb], writes=[masknegb])
            k.op("dve", lambda e: e.tensor_scalar(strict[:], self.dd[:], 0.0, None, ALU.is_gt), reads=[self.ddb], writes=[strictb])
            k.op("pool", lambda e: e.iota(Eall[:], [[1, 32], [0, 128]], base=0, channel_multiplier=-1,
                                          allow_small_or_imprecise_dtypes=True), writes=[Eallb])
            k.op("dve", lambda e: e.tensor_scalar(Eall[:], Eall[:], 0.0, None, ALU.is_equal), reads=[Eallb], writes=[Eallb])
            k.dma("sp", normw[:], dr["gdn_norm"][l], reads=[db["gdn_norm"]], writes=[normwb])
            NH = NT * 32
            beta, betab = st.sb("beta", [128, NT, 32])
            Gc, Gcb = st.sb("Gc", [128, NH])
            nGc, nGcb = st.sb("nGc", [128, NH])
            Glb, Glbb = st.sb("Glb", [128, NH])
            eG, eGb = st.sb("eG", [128, NH])
            bexp, bexpb = st.sb("bexp", [128, NH])
            kds, kdsb = st.sb("kds", [128, NH])
            cd, cdb = st.sb("cd", [128, NH])
            GT, GTb = st.sb("GT", [32, T])
            betaT, betaTb = st.sb("betaT", [32, T])
            psg = [st.ps("psg%d" % i, [128, 512]) for i in range(2)]
            with Stage(self) as st2:
                ba, bab = st2.sb("ba", [128, NT, 64])
                gg, ggb = st2.sb("gg", [128, NT, 32])
                ax, axb = st2.sb("ax", [128, NT, 32])
                nA, nAb = st2.sb("nA", [128, 32])
                dtb, dtbb = st2.sb("dtb", [128, 32])
                k.dma("sp", ba[:], dr["zba"][:, 4096:4160].rearrange("(c p) n -> p c n", p=128), reads=[db["zba"]], writes=[bab])
                k.dma("sp", nA[:], dr["gdn_alog"][l], reads=[db["gdn_alog"]], writes=[nAb])
                k.dma("sp", dtb[:], dr["gdn_dtb"][l], reads=[db["gdn_dtb"]], writes=[dtbb])
                k.op("act", lambda e: e.activation(nA[:], nA[:], AF.Exp), reads=[nAb], writes=[nAb])
                k.op("dve", lambda e: e.tensor_scalar(nA[:], nA[:], -1.0, None, ALU.mult), reads=[nAb], writes=[nAb])
                k.op("act", lambda e: e.activation(beta[:], ba[:, :, 0:32], AF.Sigmoid), reads=[bab], writes=[betab])
                for c in range(NT):
                    k.op("dve", lambda e: e.tensor_tensor(gg[:, c, :], ba[:, c, 32:64], dtb[:], ALU.add), reads=[bab, dtbb], writes=[ggb])
                k.op("act", lambda e: e.activation(ax[:], gg[:], AF.Abs), reads=[ggb], writes=[axb])
                k.op("act", lambda e: e.activation(ax[:], ax[:], AF.Exp, scale=-1.0), reads=[axb], writes=[axb])
                k.op("act", lambda e: e.activation(ax[:], ax[:], AF.Ln, bias=1.0), reads=[axb], writes=[axb])
                k.op("dve", lambda e: e.scalar_tensor_tensor(gg[:], gg[:], 0.0, ax[:], ALU.max, ALU.add), reads=[ggb, axb], writes=[ggb])
                for c in range(NT):
                    k.op("dve", lambda e: e.tensor_tensor(gg[:, c, :], gg[:, c, :], nA[:], ALU.mult), reads=[ggb, nAb], writes=[ggb])
                ggf = gg[:].rearrange("p c h -> p (c h)")
                for n0 in range(0, NH, 512):
                    nw = min(512, NH - n0)
                    ps, psb = psg[0]
                    k.op("pe", lambda e: e.matmul(ps[:, 0:nw], tri[:], ggf[:, n0:n0 + nw], start=True, stop=True),
                         reads=[trib, ggb], writes=[psb])
                    k.op("act", lambda e: e.copy(Gc[:, n0:n0 + nw], ps[:, 0:nw]), reads=[psb], writes=[Gcb])
                    ps, psb = psg[1]
                    k.op("pe", lambda e: e.matmul(ps[:, 0:nw], self.ones[:], ggf[:, n0:n0 + nw], start=True, stop=True),
                         reads=[self.onesb, ggb], writes=[psb])
                    k.op("act", lambda e: e.copy(Glb[:, n0:n0 + nw], ps[:, 0:nw]), reads=[psb], writes=[Glbb])
                for c in range(NT):
                    ps, psb = psg[c % 2]
                    k.op("pe", lambda e: e.matmul(ps[0:32, 0:128], gg[:, c, :], tri[:], start=True, stop=True),
                         reads=[ggb, trib], writes=[psb], inc=False)
                    k.op("pe", lambda e: e.matmul(ps[0:32, 128:256], beta[:, c, :], self.ident[:], start=True, stop=True),
                         reads=[betab, self.identb], writes=[psb])
                    k.op("act", lambda e: e.copy(GT[:, c * 128:(c + 1) * 128], ps[0:32, 0:128]), reads=[psb], writes=[GTb])
                    k.op("act", lambda e: e.copy(betaT[:, c * 128:(c + 1) * 128], ps[0:32, 128:256]), reads=[psb], writes=[betaTb])
            betaf = beta[:].rearrange("p c h -> p (c h)")
            k.op("dve", lambda e: e.tensor_scalar(nGc[:], Gc[:], -1.0, None, ALU.mult), reads=[Gcb], writes=[nGcb])
            k.op("act", lambda e: e.activation(eG[:], Gc[:], AF.Exp), reads=[Gcb], writes=[eGb])
            k.op("dve", lambda e: e.tensor_tensor(bexp[:], eG[:], betaf, ALU.mult), reads=[eGb, betab], writes=[bexpb])
            k.op("dve", lambda e: e.tensor_tensor(kds[:], Glb[:], Gc[:], ALU.subtract), reads=[Glbb, Gcb], writes=[kdsb])
            k.op("act", lambda e: e.activation(kds[:], kds[:], AF.Exp), reads=[kdsb], writes=[kdsb])
            k.op("act", lambda e: e.activation(cd[:], Glb[:], AF.Exp), reads=[Glbb], writes=[cdb])
            kTh, kThb = st.sb("kTh", [128, T], BF16)
            qTh, qThb = st.sb("qTh", [128, T], BF16)
            kbT, kbTb = st.sb("kbT", [128, T], BF16)
            ktk, ktkb = st.sb("ktk", [128, NT, 128], BF16)
            vtk, vtkb = st.sb("vtk", [128, NT, 128], BF16)
            zh, zhb = st.sb("zh", [128, NT, 128])
            OTh, OThb = st.sb("OTh", [128, T], BF16)
            S, Sb_ = st.sb("S", [128, 128])
            Sh, Shb = st.sb("Sh", [128, 128], BF16)
            sets = []
            for i in range(NS):
                d = {}
                for nm, shp, dt_ in (("t1", [128, 128], F32), ("DmT", [128, 128], F32),
                                     ("Nb", [128, 128], F32), ("PP0", [128, 256], F32),
                                     ("PP1", [128, 256], F32), ("RT0", [128, 128], F32), ("RT1", [128, 128], F32),
                                     ("Tinv", [128, 128], BF16),
                                     ("vb", [128, 128], BF16), ("kbg", [128, 128], BF16), ("kdec", [128, 128], BF16),
                                     ("u", [128, 128], F32), ("wTb", [128, 128], BF16), ("qkT", [128, 128], BF16)):
                    d[nm] = st.sb("%s%d" % (nm, i), shp, dt_)
                d["NTf"] = d["t1"]
                d["NTs"] = d["DmT"]
                sets.append(d)
            otl = [st.sb("otl%d" % i, [128, 128]) for i in range(2)]
            oa = [st.sb("oa%d" % i, [128, 128]) for i in range(2)]
            vnw = [st.sb("vnw%d" % i, [128, 128], BF16) for i in range(2)]
            ofin = [st.sb("ofin%d" % i, [128, 128], F32) for i in range(2)]
            sqj, sqjb = st.sb("sqj", [128, 128])
            ss = [st.sb("ss%d" % i, [128, 1]) for i in range(2)]
            psA = st.ps("psA", [128, 512])
            ps2s = [st.ps("ps2_%d" % i, [128, 512]) for i in range(2)]
            psRs = [st.ps("psR_%d" % i, [128, 512]) for i in range(2)]
            psUW, psUWb = psg[0]
            psW6, psW6b = psg[1]
            psS7, psS7b = st.ps("psS7", [128, 512])

            def pre(h, c, si):
                d = sets[si]
                cs = slice(c * 128, (c + 1) * 128)
                col = c * 32 + h
                pa, pab = psA
                p2, p2b = ps2s[c % 2]
                pr, prb = psRs[c % 2]
                t1, t1b = d["t1"]
                DmT, DmTb = d["DmT"]
                NTf, NTfb = d["NTf"]
                NTs, NTsb = d["NTs"]
                qkT, qkTb = d["qkT"]
                k.op("pe", lambda e: e.matmul(pa[:, 0:128], Eall[:, h, :], GT[:, cs], start=True, stop=True),
                     reads=[Eallb, GTb], writes=[pab], inc=False)
                k.op("pe", lambda e: e.matmul(pa[:, 128:256], kTh[:, cs], kbT[:, cs], start=True, stop=True),
                     reads=[kThb, kbTb], writes=[pab], inc=False)
                k.op("pe", lambda e: e.matmul(pa[:, 256:384], kTh[:, cs], qTh[:, cs], start=True, stop=True),
                     reads=[kThb, qThb], writes=[pab])
                k.op("dve", lambda e: e.tensor_tensor(t1[:], pa[:, 0:128], maskneg[:], ALU.add), reads=[pab, masknegb], writes=[t1b])
                k.op("act", lambda e: e.activation(DmT[:], t1[:], AF.Exp, bias=nGc[:, col:col + 1]), reads=[t1b, nGcb], writes=[DmTb])
                k.op("dve", lambda e: e.scalar_tensor_tensor(NTf[:], pa[:, 128:256], -1.0, DmT[:], ALU.mult, ALU.mult),
                     reads=[pab, DmTb], writes=[NTfb])
                k.op("dve", lambda e: e.tensor_tensor(qkT[:], pa[:, 256:384], DmT[:], ALU.mult), reads=[pab, DmTb], writes=[qkTb])
                yield
                k.op("dve", lambda e: e.tensor_tensor(NTs[:], NTf[:], strict[:], ALU.mult), reads=[NTfb, strictb], writes=[NTsb])
                yield
                RT, RTb_ = d["RT0"]
                k.op("dve", lambda e: e.tensor_tensor(RT[:], NTs[:], self.ident[:], ALU.add), reads=[NTsb, self.identb], writes=[RTb_])
                yield
                Nb, Nbb = d["Nb"]
                k.op("pe", lambda e: e.transpose(pr[:, 128:256], NTs[:], self.ident[:]), reads=[NTsb, self.identb], writes=[prb])
                k.op("act", lambda e: e.copy(Nb[:], pr[:, 128:256]), reads=[prb], writes=[Nbb])
                yield
                P_, Pb = Nb[:], Nbb
                PT_, PTb = NTs[:], NTsb
                cur = 0
                Tinv, Tinvb = d["Tinv"]
                for lv in range(6):
                    PP, PPb = d["PP%d" % (lv % 2)]
                    last = (lv == 5)
                    k.op("pe", lambda e: e.matmul(p2[:, 0:128], PT_, P_, start=True, stop=True), reads=[Pb, PTb], writes=[p2b], inc=last)
                    if not last:
                        k.op("pe", lambda e: e.matmul(p2[:, 128:256], P_, PT_, start=True, stop=True), reads=[Pb, PTb], writes=[p2b])
                    w = 128 if last else 256
                    k.op("act", lambda e: e.copy(PP[:, 0:w], p2[:, 0:w]), reads=[p2b], writes=[PPb])
                    yield
                    RTc, RTcb = d["RT%d" % cur]
                    RTn, RTnb = d["RT%d" % (1 - cur)]
                    k.op("pe", lambda e: e.matmul(pr[:, 0:128], self.ident[:], RTc[:], start=True, stop=False),
                         reads=[self.identb, RTcb], writes=[prb], inc=False)
                    k.op("pe", lambda e: e.matmul(pr[:, 0:128], PP[:, 0:128], RTc[:], start=False, stop=True),
                         reads=[PPb, RTcb], writes=[prb])
                    if last:
                        k.op("dve", lambda e: e.tensor_copy(Tinv[:], pr[:, 0:128]), reads=[prb], writes=[Tinvb])
                    else:
                        k.op("dve", lambda e: e.tensor_copy(RTn[:], pr[:, 0:128]), reads=[prb], writes=[RTnb])
                    yield
                    cur = 1 - cur
                    P_, Pb = PP[:, 0:128], PPb
                    PT_, PTb = PP[:, 128:256], PPb
                vb, vbb = d["vb"]
                kbg, kbgb = d["kbg"]
                kdec, kdecb = d["kdec"]
                k.op("dve", lambda e: e.tensor_scalar(vb[:], vtk[:, c, :], beta[:, c, h:h + 1], None, ALU.mult), reads=[vtkb, betab], writes=[vbb])
                yield
                k.op("dve", lambda e: e.tensor_scalar(kbg[:], ktk[:, c, :], bexp[:, col:col + 1], None, ALU.mult), reads=[ktkb, bexpb], writes=[kbgb])
                yield
                k.op("dve", lambda e: e.tensor_scalar(kdec[:], ktk[:, c, :], kds[:, col:col + 1], None, ALU.mult), reads=[ktkb, kdsb], writes=[kdecb])
                yield
                u, ub = d["u"]
                wTb, wTbb = d["wTb"]
                k.op("pe", lambda e: e.matmul(psUW[:, 0:128], Tinv[:], vb[:], start=True, stop=True), reads=[Tinvb, vbb], writes=[psUWb], inc=False)
                k.op("pe", lambda e: e.matmul(psUW[:, 128:256], kbg[:], Tinv[:], start=True, stop=True), reads=[Tinvb, kbgb], writes=[psUWb])
                k.op("act", lambda e: e.copy(u[:], psUW[:, 0:128]), reads=[psUWb], writes=[ub])
                k.op("act", lambda e: e.copy(wTb[:], psUW[:, 128:256]), reads=[psUWb], writes=[wTbb])
                yield

            def rec(h, c, si):
                d = sets[si]
                cs = slice(c * 128, (c + 1) * 128)
                col = c * 32 + h
                u, ub = d["u"]
                wTb, wTbb = d["wTb"]
                kdec, kdecb = d["kdec"]
                qkT, qkTb = d["qkT"]
                vn, vnb = vnw[c % 2]
                ot, otb = otl[c % 2]
                oa_, oab = oa[c % 2]
                k.op("pe", lambda e: e.matmul(psW6[:, 0:128], wTb[:], Sh[:], start=True, stop=True), reads=[wTbb, Shb], writes=[psW6b], inc=False)
                k.op("pe", lambda e: e.matmul(psW6[:, 128:256], qTh[:, cs], Sh[:], start=True, stop=True), reads=[qThb, Shb], writes=[psW6b])
                yield
                k.op("dve", lambda e: e.tensor_tensor(vn[:], u[:], psW6[:, 0:128], ALU.subtract), reads=[ub, psW6b], writes=[vnb])
                yield
                k.op("pe", lambda e: e.matmul(psS7[:, 0:128], kdec[:], vn[:], start=True, stop=True), reads=[kdecb, vnb], writes=[psS7b], inc=False)
                k.op("pe", lambda e: e.matmul(psS7[:, 128:256], qkT[:], vn[:], start=True, stop=True), reads=[qkTb, vnb], writes=[psS7b])
                yield
                k.op("dve", lambda e: e.scalar_tensor_tensor(S[:], S[:], cd[:, col:col + 1], psS7[:, 0:128], ALU.mult, ALU.add),
                     reads=[Sb_, cdb, psS7b], writes=[Sb_])
                yield
                k.op("act", lambda e: e.copy(Sh[:], S[:]), reads=[Sb_], writes=[Shb])
                yield
                k.op("act", lambda e: e.activation(oa_[:], psW6[:, 128:256], AF.Copy, scale=eG[:, col:col + 1]), reads=[psW6b, eGb], writes=[oab])
                yield
                k.op("dve", lambda e: e.tensor_tensor(ot[:], oa_[:], psS7[:, 128:256], ALU.add), reads=[oab, psS7b], writes=[otb])
                yield
                s_, s_b = ss[c % 2]
                k.op("act", lambda e: e.activation(sqj[:], ot[:], AF.Square, accum_out=s_[:]), reads=[otb], writes=[sqjb, s_b])
                yield
                k.op("act", lambda e: e.activation(s_[:], s_[:], AF.Sqrt, bias=1e-6, scale=1.0 / 128.0), reads=[s_b], writes=[s_b])
                k.op("dve", lambda e: e.reciprocal(s_[:], s_[:]), reads=[s_b], writes=[s_b])
                yield
                k.op("dve", lambda e: e.scalar_tensor_tensor(ot[:], ot[:], s_[:, 0:1], normw[:], ALU.mult, ALU.mult),
                     reads=[otb, s_b, normwb], writes=[otb])
                yield
                of, ofb = ofin[c % 2]
                k.op("dve", lambda e: e.tensor_tensor(of[:], ot[:], zh[:, c, :], ALU.mult), reads=[otb, zhb], writes=[ofb])
                yield
                k.op("pe", lambda e: e.transpose(psS7[:, 256:384], of[:], self.ident[:]), reads=[ofb, self.identb], writes=[psS7b])
                yield
                k.op("act", lambda e: e.copy(OTh[:, cs], psS7[:, 256:384]), reads=[psS7b], writes=[OThb])
                yield

            def run_interleaved(gens_a, gens_b, ratio):
                ia = iter(gens_a)
                ga = next(ia, None)
                pool = list(gens_b)
                rr = 0
                while ga is not None or pool:
                    if ga is not None:
                        try:
                            next(ga)
                        except StopIteration:
                            ga = next(ia, None)
                    for _ in range(ratio if ga is not None else max(1, len(pool))):
                        if not pool:
                            break
                        rr %= len(pool)
                        try:
                            next(pool[rr])
                            rr += 1
                        except StopIteration:
                            pool.pop(rr)

            BATCH = 4
            for h in range(32):
                hq = h // 2
                if h % 2 == 0:
                    k.dma("sp", kTh[:], dr["kT"][hq * 128:(hq + 1) * 128, :], reads=[db["kT"]], writes=[kThb])
                    k.dma("sp", qTh[:], dr["qT"][hq * 128:(hq + 1) * 128, :], reads=[db["qT"]], writes=[qThb])
                    k.dma("sp", ktk[:], dr["ktok"][:, hq * 128:(hq + 1) * 128].rearrange("(c p) e -> p c e", p=128),
                          reads=[db["ktok"]], writes=[ktkb])
                k.dma("sp", vtk[:], dr["vtok"][:, h * 128:(h + 1) * 128].rearrange("(c p) e -> p c e", p=128),
                      reads=[db["vtok"]], writes=[vtkb])
                k.dma("sp", zh[:], dr["zba"][:, h * 128:(h + 1) * 128].rearrange("(c p) e -> p c e", p=128),
                      reads=[db["zba"]], writes=[zhb])
                k.op("act", lambda e: e.activation(zh[:], zh[:], AF.Silu), reads=[zhb], writes=[zhb])
                for tg in range(T // 512):
                    sl = slice(tg * 512, (tg + 1) * 512)
                    pa, pab = ps2s[tg % 2]
                    k.op("pe", lambda e: e.matmul(pa[:], Eall[:, h, :], betaT[:, sl], start=True, stop=True),
                         reads=[Eallb, betaTb], writes=[pab])
                    k.op("dve", lambda e: e.tensor_tensor(kbT[:, sl], kTh[:, sl], pa[:], ALU.mult), reads=[kThb, pab], writes=[kbTb])
                k.op("dve", lambda e: e.memset(S[:], 0.0), writes=[Sb_])
                k.op("dve", lambda e: e.memset(Sh[:], 0.0), writes=[Shb])
                nb_ = NT // BATCH
                for b in range(nb_ + 1):
                    recs = [rec(h, c, c % NS) for c in range((b - 1) * BATCH, b * BATCH)] if b >= 1 else []
                    pres = [pre(h, c, c % NS) for c in range(b * BATCH, (b + 1) * BATCH)] if b < nb_ else []
                    run_interleaved(recs, pres, 4)
                k.dma("sp", dr["OT"][h * 128:(h + 1) * 128, :], OTh[:], reads=[OThb], writes=[db["OT"]])


    def declare_attn(self):
        T = self.T
        self.din("kv_w_k", [D, 1024])
        self.din("kv_w_v", [D, 1024])
        self.din("dil_w_q", [2, D, 3072])
        self.din("dil_w_o", [2, 1024, D])
        self.din("biasT", [128, 24, 256])
        self.dscr("KT", [1024, T], BF16)
        self.dscr("V", [T, 1024], BF16)
        self.dscr("QT", [3072, T], BF16)
        self.dscr("OG", [3, T, 1024])
        self.dscr("LSE", [3, T, 8])
        self.dscr("OT2", [1024, T], BF16)

    def mm_feat(self, W, Wb, N, OUT, OUTb, scale):
        k = self.k
        T = self.T
        dr, db = self.dr, self.db
        TH = min(T, 2048)
        nh = T // TH
        ntg = TH // 512
        nbn = N // 128
        with Stage(self) as st:
            xT, xTb = st.sb("xT", [128, 16, TH], BF16)
            wts = [st.sb("w%d" % i, [128, 16, 128], BF16) for i in range(2)]
            obs = [st.sb("ob%d" % i, [128, TH], BF16) for i in range(2)]
            pss = [st.ps("ps%d" % i, [128, 512]) for i in range(2)]

            def load_w(j):
                nb = j % nbn
                wt, wtb = wts[j % 2]
                for c0 in (0, 8):
                    src = W[c0 * 128:(c0 + 8) * 128, nb * 128:(nb + 1) * 128].rearrange("(kc p) n -> p kc n", p=128)
                    k.dma("pool", wt[:, c0:c0 + 8, :], src, reads=[Wb], writes=[wtb])

            j = 0
            pi = 0
            load_w(0)
            for half in range(nh):
                t0 = half * TH
                for kc in range(16):
                    k.dma("sp", xT[:, kc, :], dr["HT"][kc * 128:(kc + 1) * 128, t0:t0 + TH], reads=[db["HT"]], writes=[xTb])
                for nb in range(nbn):
                    if j + 1 < nh * nbn:
                        load_w(j + 1)
                    wt, wtb = wts[j % 2]
                    ob, obb = obs[j % 2]
                    j += 1
                    for tg in range(ntg):
                        ps, psb = pss[pi % 2]
                        pi += 1
                        for kc in range(16):
                            k.op("pe", lambda e: e.matmul(ps[:], wt[:, kc, :], xT[:, kc, tg * 512:(tg + 1) * 512],
                                                          start=(kc == 0), stop=(kc == 15)),
                                 reads=[wtb, xTb], writes=[psb], inc=(kc == 15))
                        if tg % 2 == 0:
                            k.op("act", lambda e: e.activation(ob[:, tg * 512:(tg + 1) * 512], ps[:], AF.Copy, scale=scale), reads=[psb], writes=[obb])
                        else:
                            k.op("dve", lambda e: e.tensor_scalar(ob[:, tg * 512:(tg + 1) * 512], ps[:], scale, None, ALU.mult), reads=[psb], writes=[obb])
                    k.dma("sp", OUT[nb * 128:(nb + 1) * 128, t0:t0 + TH], ob[:], reads=[obb], writes=[OUTb])

    def attn_core(self):
        k = self.k
        T = self.T
        dr, db = self.dr, self.db
        with Stage(self) as st:
            KT, KTb = st.sb("KT", [128, 8, T], BF16)
            QT, QTb = st.sb("QT", [128, 8, T], BF16)
            biasm, biasmb = st.sb("biasm", [128, 24, 256], const=True)
            mk, mkb = st.sb("mk", [128, 256])
            mk2, mk2b = st.sb("mk2", [128, 256])
            vts = [st.sb("vt%d" % i, [128, 1024], BF16) for i in range(3)]
            ss_ = [st.sb("s%d" % i, [128, 256]) for i in range(2)]
            pp = [st.sb("p%d" % i, [128, 256], BF16) for i in range(2)]
            pT = [st.sb("pT%d" % i, [128, 256], BF16) for i in range(2)]
            ob_ = [st.sb("ob%d" % i, [128, 8, 128]) for i in range(2)]
            lb_ = [st.sb("lb%d" % i, [128, 8]) for i in range(2)]
            mm_ = [st.sb("m%d" % i, [128, 1]) for i in range(4)]
            dn_ = [st.sb("dn%d" % i, [128, 1]) for i in range(4)]
            pss = [st.ps("pss%d" % i, [128, 512]) for i in range(2)]
            pst = [st.ps("pst%d" % i, [128, 1024], BF16) for i in range(2)]
            pso = [st.ps("pso%d" % i, [128, 512]) for i in range(2)]
            k.op("pool", lambda e: e.iota(mk[:], [[-1, 256]], base=128, channel_multiplier=1, allow_small_or_imprecise_dtypes=True), writes=[mkb])
            k.op("dve", lambda e: e.tensor_scalar(mk2[:], mk[:], 0.0, NEG, ALU.is_lt, ALU.mult), reads=[mkb], writes=[mk2b])
            k.op("dve", lambda e: e.tensor_scalar(mk[:], mk[:], 128.0, NEG, ALU.is_gt, ALU.mult), reads=[mkb], writes=[mkb])
            k.op("dve", lambda e: e.tensor_tensor(mk[:], mk[:], mk2[:], ALU.add), reads=[mkb, mk2b], writes=[mkb])
            k.dma("sp", biasm[:], dr["biasT"], reads=[db["biasT"]], writes=[biasmb])
            for i in range(24):
                k.op("dve", lambda e: e.tensor_tensor(biasm[:, i, :], biasm[:, i, :], mk[:], ALU.add), reads=[biasmb, mkb], writes=[biasmb])
            for h in range(8):
                k.dma("sp", KT[:, h, :], dr["KT"][h * 128:(h + 1) * 128, :], reads=[db["KT"]], writes=[KTb])
            cnt = 0
            vi = 0
            for g, dil in enumerate((1, 4, 16)):
                for h in range(8):
                    k.dma("sp", QT[:, h, :], dr["QT"][(g * 8 + h) * 128:(g * 8 + h + 1) * 128, :], reads=[db["QT"]], writes=[QTb])
                L = T // dil
                nblk = L // 128
                for r in range(dil):
                    vprev = None
                    for n in range(nblk):
                        vt, vtb = vts[vi % 3]
                        vi += 1
                        start = n * 128 * dil + r
                        rows = dr["V"][start:start + 127 * dil + 1:dil, :] if dil > 1 else dr["V"][start:start + 128, :]
                        k.dma("sp", vt[:], rows, reads=[db["V"]], writes=[vtb])
                        ob, obb = ob_[cnt % 2]
                        lb, lbb = lb_[cnt % 2]
                        cnt += 1
                        for h in range(8):
                            ps, psb = pss[h % 2]
                            s, sb_ = ss_[h % 2]
                            p, pb = pp[h % 2]
                            ptt, pttb = pT[h % 2]
                            pt, ptb = pst[h % 2]
                            po, pob = pso[h % 2]
                            m, mb = mm_[h % 4]
                            dn, dnb = dn_[h % 4]
                            qs = QT[:, h, start:start + 127 * dil + 1:dil] if dil > 1 else QT[:, h, start:start + 128]
                            if n == 0:
                                w0, nw = 128, 128
                                ks = KT[:, h, start:start + 127 * dil + 1:dil] if dil > 1 else KT[:, h, start:start + 128]
                            else:
                                w0, nw = 0, 256
                                ps0 = start - 128 * dil
                                ks = KT[:, h, ps0:ps0 + 255 * dil + 1:dil] if dil > 1 else KT[:, h, ps0:ps0 + 256]
                            k.op("pe", lambda e: e.matmul(ps[:, 0:nw], qs, ks, start=True, stop=True), reads=[QTb, KTb], writes=[psb])
                            k.op("dve", lambda e: e.tensor_tensor(s[:, 0:nw], ps[:, 0:nw], biasm[:, g * 8 + h, w0:w0 + nw], ALU.add),
                                 reads=[psb, biasmb], writes=[sb_])
                            k.op("dve", lambda e: e.tensor_reduce(m[:], s[:, 0:nw], AX.X, ALU.max), reads=[sb_], writes=[mb])
                            k.op("dve", lambda e: e.tensor_scalar(m[:], m[:], -1.0, None, ALU.mult), reads=[mb], writes=[mb])
                            k.op("act", lambda e: e.activation(p[:, 0:nw], s[:, 0:nw], AF.Exp, bias=m[:, 0:1], accum_out=dn[:]),
                                 reads=[sb_, mb], writes=[pb, dnb])
                            nparts = nw // 128
                            for q_ in range(nparts):
                                k.op("pe", lambda e: e.transpose(pt[:, q_ * 128:(q_ + 1) * 128], p[:, q_ * 128:(q_ + 1) * 128], self.identh[:]),
                                     reads=[pb, self.identhb], writes=[ptb], inc=(q_ == nparts - 1))
                            k.op("act", lambda e: e.copy(ptt[:, 0:nw], pt[:, 0:nw]), reads=[ptb], writes=[pttb])
                            hs = slice(h * 128, (h + 1) * 128)
                            if n == 0:
                                k.op("pe", lambda e: e.matmul(po[:, 0:128], ptt[:, 0:128], vt[:, hs], start=True, stop=True),
                                     reads=[pttb, vtb], writes=[pob])
                            else:
                                k.op("pe", lambda e: e.matmul(po[:, 0:128], ptt[:, 0:128], vprev[0][:, hs], start=True, stop=False),
                                     reads=[pttb, vprev[1]], writes=[pob], inc=False)
                                k.op("pe", lambda e: e.matmul(po[:, 0:128], ptt[:, 128:256], vt[:, hs], start=False, stop=True),
                                     reads=[pttb, vtb], writes=[pob])
                            k.op("act", lambda e: e.activation(lb[:, h:h + 1], dn[:], AF.Ln), reads=[dnb], writes=[lbb])
                            k.op("dve", lambda e: e.tensor_tensor(lb[:, h:h + 1], lb[:, h:h + 1], m[:], ALU.subtract), reads=[lbb, mb], writes=[lbb])
                            k.op("dve", lambda e: e.reciprocal(dn[:], dn[:]), reads=[dnb], writes=[dnb])
                            k.op("act", lambda e: e.activation(ob[:, h, :], po[:, 0:128], AF.Copy, scale=dn[:, 0:1]), reads=[pob, dnb], writes=[obb])
                        orows = dr["OG"][g, start:start + 127 * dil + 1:dil, :] if dil > 1 else dr["OG"][g, start:start + 128, :]
                        lrows = dr["LSE"][g, start:start + 127 * dil + 1:dil, :] if dil > 1 else dr["LSE"][g, start:start + 128, :]
                        k.dma("sp", orows, ob[:].rearrange("p h e -> p (h e)"), reads=[obb], writes=[db["OG"]])
                        k.dma("sp", lrows, lb[:], reads=[lbb], writes=[db["LSE"]])
                        vprev = (vt, vtb)

    def attn_combine(self):
        k = self.k
        T = self.T
        dr, db = self.dr, self.db
        with Stage(self) as st:
            ogs = [[st.sb("og%d_%d" % (g, i), [128, 8, 128]) for g in range(3)] for i in range(2)]
            lss = [[st.sb("ls%d_%d" % (g, i), [128, 8]) for g in range(3)] for i in range(2)]
            M, Mb = st.sb("M", [128, 8])
            sm, smb = st.sb("sm", [128, 8])
            oc, ocb = st.sb("oc", [128, 8, 128])
            och, ochb = st.sb("och", [128, 1024], BF16)
            oT, oTb = st.sb("oT", [128, 8, 512], BF16)
            pst, pstb = st.ps("pst", [128, 1024], BF16)
            for ti in range(self.NT):
                og = ogs[ti % 2]
                ls = lss[ti % 2]
                rs = slice(ti * 128, (ti + 1) * 128)
                for g in range(3):
                    k.dma("sp", og[g][0][:].rearrange("p h e -> p (h e)"), dr["OG"][g, rs, :], reads=[db["OG"]], writes=[og[g][1]])
                    k.dma("sp", ls[g][0][:], dr["LSE"][g, rs, :], reads=[db["LSE"]], writes=[ls[g][1]])
                k.op("dve", lambda e: e.tensor_tensor(M[:], ls[0][0][:], ls[1][0][:], ALU.max), reads=[ls[0][1], ls[1][1]], writes=[Mb])
                k.op("dve", lambda e: e.tensor_tensor(M[:], M[:], ls[2][0][:], ALU.max), reads=[Mb, ls[2][1]], writes=[Mb])
                for g in range(3):
                    k.op("dve", lambda e: e.tensor_tensor(ls[g][0][:], ls[g][0][:], M[:], ALU.subtract), reads=[ls[g][1], Mb], writes=[ls[g][1]])
                    k.op("act", lambda e: e.activation(ls[g][0][:], ls[g][0][:], AF.Exp), reads=[ls[g][1]], writes=[ls[g][1]])
                k.op("dve", lambda e: e.tensor_tensor(sm[:], ls[0][0][:], ls[1][0][:], ALU.add), reads=[ls[0][1], ls[1][1]], writes=[smb])
                k.op("dve", lambda e: e.tensor_tensor(sm[:], sm[:], ls[2][0][:], ALU.add), reads=[smb, ls[2][1]], writes=[smb])
                k.op("dve", lambda e: e.reciprocal(sm[:], sm[:]), reads=[smb], writes=[smb])
                for g in range(3):
                    k.op("dve", lambda e: e.tensor_tensor(ls[g][0][:], ls[g][0][:], sm[:], ALU.mult), reads=[ls[g][1], smb], writes=[ls[g][1]])
                for h in range(8):
                    k.op("dve", lambda e: e.tensor_scalar(oc[:, h, :], og[0][0][:, h, :], ls[0][0][:, h:h + 1], None, ALU.mult),
                         reads=[og[0][1], ls[0][1]], writes=[ocb])
                    for g in (1, 2):
                        k.op("dve", lambda e: e.scalar_tensor_tensor(oc[:, h, :], og[g][0][:, h, :], ls[g][0][:, h:h + 1], oc[:, h, :], ALU.mult, ALU.add),
                             reads=[og[g][1], ls[g][1], ocb], writes=[ocb])
                k.op("act", lambda e: e.copy(och[:], oc[:].rearrange("p h e -> p (h e)")), reads=[ocb], writes=[ochb])
                for h in range(8):
                    k.op("pe", lambda e: e.transpose(pst[:, h * 128:(h + 1) * 128], och[:, h * 128:(h + 1) * 128], self.identh[:]),
                         reads=[ochb, self.identhb], writes=[pstb], inc=(h == 7))
                q = ti % 4
                k.op("dve", lambda e: e.tensor_copy(oT[:, :, q * 128:(q + 1) * 128], pst[:].rearrange("p (h e) -> p h e", h=8)), reads=[pstb], writes=[oTb])
                if q == 3:
                    tg = ti // 4
                    dst = dr["OT2"][:, tg * 512:(tg + 1) * 512].rearrange("(kc p) t -> p kc t", p=128)
                    k.dma("sp", dst, oT[:], reads=[oTb], writes=[db["OT2"]])

    def declare_common(self):
        T = self.T
        self.din("ln_gain", [DEPTH, 2, 128, D])
        self.din("ln_bias", [DEPTH, 2, 128, D])
        self.din("w_router", [DEPTH, D, 36])
        self.din("b_router", [DEPTH, 128, 36])
        self.din("moe_w1", [DEPTH, NE, D, 512])
        self.din("moe_w3", [DEPTH, NE, D, 512])
        self.din("moe_w2", [DEPTH, NE, 512, D])
        self.dscr("H", [T, D])
        self.Hb = [Buf("H%d" % i) for i in range(self.NT)]
        self.dscr("HT", [D, T], BF16)
        self.dscr("Y", [T, D])
        self.dscr("G", [T, 32])
        self.dscr("out", [T, D], out=True)

    def copy_dram(self, src, srcb, dst, dstb, rows, cols, dtype=F32):
        k = self.k
        with Stage(self) as st:
            bufs = [st.sb("cp%d" % i, [128, cols], dtype) for i in range(2)]
            for i in range(rows // 128):
                t, tb = bufs[i % 2]
                k.dma("sp", t[:], src[i * 128:(i + 1) * 128, :], reads=[srcb], writes=[tb])
                k.dma("sp", dst[i * 128:(i + 1) * 128, :], t[:], reads=[tb], writes=[dstb[i] if isinstance(dstb, list) else dstb])

    def to_HT(self):
        k = self.k
        dr, db = self.dr, self.db
        with Stage(self) as st:
            hs = [st.sb("h%d" % i, [128, D]) for i in range(2)]
            xTb, xTbb = st.sb("xTb", [128, 16, 512], BF16)
            pst = [st.ps("pst%d" % i, [128, 512]) for i in range(2)]
            for ti in range(self.NT):
                hh, hhb = hs[ti % 2]
                k.dma("sp", hh[:], dr["H"][ti * 128:(ti + 1) * 128, :], reads=[self.Hb[ti]], writes=[hhb])
                q = ti % 4
                for c4 in range(4):
                    ps, psb = pst[c4 % 2]
                    for c in range(4):
                        kc = c4 * 4 + c
                        k.op("pe", lambda e: e.transpose(ps[:, c * 128:(c + 1) * 128], hh[:, kc * 128:(kc + 1) * 128], self.ident[:]),
                             reads=[hhb, self.identb], writes=[psb], inc=(c == 3))
                    k.op("dve" if c4 % 2 else "act",
                         (lambda e: e.tensor_copy(xTb[:, c4 * 4:(c4 + 1) * 4, q * 128:(q + 1) * 128], ps[:].rearrange("p (a b) -> p a b", a=4)))
                         if c4 % 2 else
                         (lambda e: e.copy(xTb[:, c4 * 4:(c4 + 1) * 4, q * 128:(q + 1) * 128], ps[:].rearrange("p (a b) -> p a b", a=4))),
                         reads=[psb], writes=[xTbb])
                if q == 3:
                    tg = ti // 4
                    dst = dr["HT"][:, tg * 512:(tg + 1) * 512].rearrange("(kc p) t -> p kc t", p=128)
                    k.dma("sp", dst, xTb[:], reads=[xTbb], writes=[db["HT"]])

    def finish(self):
        self.k.barrier()
        self.gstack.close()


def build_moe_test(T, full=False):
    P = Prog(T, mode="moe_test", dbg=("H", "G", "HT"))
    P.declare_common()
    x = P.din("x", [T, D])
    yin = P.din("yin", [T, D])
    P.setup_consts()
    P.copy_dram(x, P.db["x"], P.dr["H"], P.Hb, T, D)
    P.copy_dram(yin, P.db["yin"], P.dr["Y"], P.db["Y"], T, D)
    P.ln_stage(0, 0, "Y", True)
    if full:
        P.moe_stage(0)
    P.finish()
    return P


def prep_common(inp):
    d = {}
    d["ln_gain"] = np.ascontiguousarray(np.broadcast_to(inp["ln_gain"][:, :, None, :], (DEPTH, 2, 128, D))).astype(np.float32)
    d["ln_bias"] = np.ascontiguousarray(np.broadcast_to(inp["ln_bias"][:, :, None, :], (DEPTH, 2, 128, D))).astype(np.float32)
    d["w_router"] = np.ascontiguousarray(np.concatenate([inp["moe_w_group"], inp["moe_w_expert"]], axis=-1)).astype(np.float32)
    br = np.concatenate([inp["moe_b_group"], inp["moe_b_expert"]], axis=-1)
    d["b_router"] = np.ascontiguousarray(np.broadcast_to(br[:, None, :], (DEPTH, 128, 36))).astype(np.float32)
    for n in ("moe_w1", "moe_w3", "moe_w2"):
        d[n] = np.ascontiguousarray(inp[n])
    return d


def prep_gdn(inp):
    d = {}
    d["gdn_w_in"] = np.ascontiguousarray(inp["gdn_w_in"])
    cw = inp["gdn_conv"]
    d["gdn_conv"] = np.ascontiguousarray(cw.reshape(NA, 4, 64, 128).transpose(0, 2, 3, 1))
    d["gdn_alog"] = np.ascontiguousarray(np.broadcast_to(inp["gdn_a_log"][:, None, :], (NA, 128, 32))).astype(np.float32)
    d["gdn_dtb"] = np.ascontiguousarray(np.broadcast_to(inp["gdn_dt_bias"][:, None, :], (NA, 128, 32))).astype(np.float32)
    d["gdn_norm"] = np.ascontiguousarray(np.broadcast_to(inp["gdn_norm"][:, None, :], (NA, 128, 128))).astype(np.float32)
    d["gdn_w_out"] = np.ascontiguousarray(inp["gdn_w_out"])
    return d


def build_gdn_test(T, upto=1):
    P = Prog(T, dbg=("qT", "kT", "ktok", "vtok", "zba", "OT", "Y", "HT"))
    P.declare_common()
    P.declare_gdn()
    x = P.din("x", [T, D])
    P.setup_consts()
    P.copy_dram(x, P.db["x"], P.dr["H"], P.Hb, T, D)
    P.to_HT()
    P.gdn_qkv(0)
    if upto >= 2:
        P.mm_tok("zba", P.dr["HT"], P.db["HT"], D, P.dr["gdn_w_in"][0][:, 8192:GDN_IN], P.db["gdn_w_in"], 4160, P.dr["zba"], P.db["zba"])
    if upto >= 3:
        P.gdn_core(0)
    if upto >= 4:
        P.mm_tok("oproj", P.dr["OT"], P.db["OT"], 4096, P.dr["gdn_w_out"][0], P.db["gdn_w_out"], D, P.dr["Y"], P.db["Y"])
    P.finish()
    return P


def t5_bias_table(rel_bias):
    qi = np.arange(128)[:, None]
    kj = np.arange(256)[None, :]
    steps = np.maximum(128 + qi - kj, 0)
    out = np.zeros((128, 24, 256), np.float32)
    for g, dil in enumerate((1, 4, 16)):
        dist = steps * dil
        distf = np.maximum(dist, 16).astype(np.float32)
        large = 16 + (np.log(distf / np.float32(16)) / np.float32(np.log(2048 / 16)) * np.float32(16)).astype(np.int32)
        bucket = np.where(dist < 16, dist, np.minimum(large, 31))
        for h in range(8):
            out[:, g * 8 + h, :] = rel_bias[bucket, g * 8 + h]
    return out


def prep_attn(inp):
    d = {}
    for n in ("kv_w_k", "kv_w_v", "dil_w_q", "dil_w_o"):
        d[n] = np.ascontiguousarray(inp[n])
    d["biasT"] = t5_bias_table(np.asarray(inp["rel_bias"]))
    return d


def build_attn_test(T):
    P = Prog(T, dbg=("KT", "V", "QT", "OG", "LSE", "OT2"))
    P.declare_common()
    P.declare_attn()
    x = P.din("x", [T, D])
    P.setup_consts()
    P.copy_dram(x, P.db["x"], P.dr["H"], P.Hb, T, D)
    P.to_HT()
    P.mm_feat(P.dr["kv_w_k"], P.db["kv_w_k"], 1024, P.dr["KT"], P.db["KT"], 1.0)
    P.mm_tok("v", P.dr["HT"], P.db["HT"], D, P.dr["kv_w_v"], P.db["kv_w_v"], 1024, P.dr["V"], P.db["V"], out_dtype=BF16)
    P.mm_feat(P.dr["dil_w_q"][0], P.db["dil_w_q"], 3072, P.dr["QT"], P.db["QT"], 128.0 ** -0.5)
    P.attn_core()
    P.attn_combine()
    P.finish()
    return P


def build_full(T):
    P = Prog(T)
    P.declare_common()
    P.declare_gdn()
    P.declare_attn()
    x = P.din("x", [T, D])
    P.setup_consts()
    P.copy_dram(x, P.db["x"], P.dr["H"], P.Hb, T, D)
    P.to_HT()
    dr, db = P.dr, P.db
    for l in range(NA):
        P.gdn_qkv(l)
        P.mm_tok("zba", dr["HT"], db["HT"], D, dr["gdn_w_in"][l][:, 8192:GDN_IN], db["gdn_w_in"], 4160, dr["zba"], db["zba"])
        P.gdn_core(l)
        P.mm_tok("oproj", dr["OT"], db["OT"], 4096, dr["gdn_w_out"][l], db["gdn_w_out"], D, dr["Y"], db["Y"])
        P.ln_stage(l, 0, "Y", True)
        P.moe_stage(l)
    P.mm_feat(dr["kv_w_k"], db["kv_w_k"], 1024, dr["KT"], db["KT"], 1.0)
    P.mm_tok("v", dr["HT"], db["HT"], D, dr["kv_w_v"], db["kv_w_v"], 1024, dr["V"], db["V"], out_dtype=BF16)
    for j in range(DEPTH - NA):
        l = NA + j
        P.mm_feat(dr["dil_w_q"][j], db["dil_w_q"], 3072, dr["QT"], db["QT"], 128.0 ** -0.5)
        P.attn_core()
        P.attn_combine()
        P.mm_tok("wo", dr["OT2"], db["OT2"], 1024, dr["dil_w_o"][j], db["dil_w_o"], D, dr["Y"], db["Y"])
        P.ln_stage(l, 0, "Y", True)
        P.moe_stage(l)
    P.finish()
    return P


def kernel(**inputs):
    inp = {k_: np.asarray(v) for k_, v in inputs.items()}
    B, T, _ = inp["x"].shape
    shared = {}
    shared.update(prep_common(inp))
    shared.update(prep_gdn(inp))
    shared.update(prep_attn(inp))
    P = build_full(T)
    in_maps = []
    for b in range(B):
        m = dict(shared)
        m["x"] = np.ascontiguousarray(inp["x"][b]).astype(np.float32)
        in_maps.append(m)
    res = run_bass_kernel_spmd(P.nc, in_maps, core_ids=list(range(B)))
    out = np.stack([np.asarray(res.results[b]["out"]) for b in range(B)], axis=0)
    return out.astype(np.float32)
```
